# Optimizing a Trainium2 kernel written in Bass

```python
import jax, jax.numpy as jnp
from jax import lax
import numpy as np

D_MODEL = 1024
BATCH = 16
SEQ = 2048
DEPTH = 2

HEAD_DIM = 64
MOBA_HEADS = 8
MOBA_BLOCK = 256
MOBA_TOPK = 3
MOBA_Q_CHUNK = 16
DIL_PATTERNS = ((128, 1), (512, 4), (2048, 16))
DIL_HEADS_PER_GROUP = 4
N_DIL_GROUPS = 3
ROPE_THETA = 10000.0
RMS_EPS = 1e-6
D_FF_DENSE = 2816
N_EXPERTS = 8
TOP_K = 2
D_FF_EXPERT = 3584

WIDTH_A = MOBA_HEADS * HEAD_DIM
WIDTH_B = N_DIL_GROUPS * DIL_HEADS_PER_GROUP * HEAD_DIM
WIDTH_B_OUT = DIL_HEADS_PER_GROUP * HEAD_DIM
IN_PROJ_COLS = 3 * WIDTH_A + 3 * WIDTH_B + 2 * D_MODEL

kernel_name = 'hybrid_moba_dilated_gated_moe'


def rms_norm(x, g):
    xf = x.astype(jnp.float32)
    y = xf * lax.rsqrt(jnp.mean(xf * xf, axis=-1, keepdims=True) + RMS_EPS)
    return (y * g.astype(jnp.float32)).astype(x.dtype)


def rope(x, pos):
    half = x.shape[-1] // 2
    inv = ROPE_THETA ** (-jnp.arange(half, dtype=jnp.float32) / half)
    ang = pos.astype(jnp.float32)[:, None] * inv[None, :]
    cos = jnp.cos(ang)[None, :, None, :]
    sin = jnp.sin(ang)[None, :, None, :]
    x1 = x[..., :half].astype(jnp.float32)
    x2 = x[..., half:].astype(jnp.float32)
    return jnp.concatenate([x1 * cos - x2 * sin, x2 * cos + x1 * sin], axis=-1).astype(x.dtype)


def moba_attention(q, k, v):
    bsz, seq, n_heads, head_dim = q.shape
    n_blk = -(-seq // MOBA_BLOCK)
    seq_p = n_blk * MOBA_BLOCK
    pad = ((0, 0), (0, seq_p - seq), (0, 0), (0, 0))
    qh = jnp.pad(q, pad).transpose(0, 2, 1, 3)
    k_blk = jnp.pad(k, pad).transpose(0, 2, 1, 3).reshape(bsz, n_heads, n_blk, MOBA_BLOCK, head_dim)
    v_blk = jnp.pad(v, pad).transpose(0, 2, 1, 3).reshape(bsz, n_heads, n_blk, MOBA_BLOCK, head_dim)
    scale = head_dim ** -0.5
    q_blk_id = jnp.arange(seq_p) // MOBA_BLOCK
    n_sel = min(MOBA_TOPK, n_blk - 1)
    if n_sel > 0:
        k_mean = jnp.mean(k_blk.astype(jnp.float32), axis=3)
        gate = jnp.einsum('bhsd,bhnd->bhsn', qh.astype(jnp.float32), k_mean)
        fully_past = jnp.arange(n_blk)[None, :] < q_blk_id[:, None]
        gate = jnp.where(fully_past, gate, -jnp.inf)
        _, sel = lax.top_k(gate, n_sel)
        sel_ok = sel < q_blk_id[:, None]
    gather = jax.vmap(jax.vmap(lambda kb, ix: kb[ix]))
    local = jnp.arange(MOBA_BLOCK)

    def chunk(c):
        start = c * MOBA_Q_CHUNK
        blk = start // MOBA_BLOCK
        qc = lax.dynamic_slice_in_dim(qh, start, MOBA_Q_CHUNK, axis=2)
        k_own = lax.dynamic_index_in_dim(k_blk, blk, axis=2, keepdims=False)
        v_own = lax.dynamic_index_in_dim(v_blk, blk, axis=2, keepdims=False)
        q_pos = start + jnp.arange(MOBA_Q_CHUNK)
        k_pos = blk * MOBA_BLOCK + local
        s_own = jnp.einsum('bhqd,bhkd->bhqk', qc, k_own).astype(jnp.float32) * scale
        s_own = jnp.where(k_pos[None, :] <= q_pos[:, None], s_own, -jnp.inf)
        if n_sel == 0:
            p = jax.nn.softmax(s_own, axis=-1).astype(v.dtype)
            return jnp.einsum('bhqk,bhkd->bhqd', p, v_own)
        sel_c = lax.dynamic_slice_in_dim(sel, start, MOBA_Q_CHUNK, axis=2)
        ok_c = lax.dynamic_slice_in_dim(sel_ok, start, MOBA_Q_CHUNK, axis=2)
        k_sel = gather(k_blk, sel_c)
        v_sel = gather(v_blk, sel_c)
        s_sel = jnp.einsum('bhqd,bhqnkd->bhqnk', qc, k_sel).astype(jnp.float32) * scale
        s_sel = jnp.where(ok_c[..., None], s_sel, -jnp.inf)
        n_past = n_sel * MOBA_BLOCK
        s = jnp.concatenate([s_sel.reshape(bsz, n_heads, MOBA_Q_CHUNK, n_past), s_own], axis=-1)
        p = jax.nn.softmax(s, axis=-1).astype(v.dtype)
        p_sel = p[..., :n_past].reshape(bsz, n_heads, MOBA_Q_CHUNK, n_sel, MOBA_BLOCK)
        return (jnp.einsum('bhqnk,bhqnkd->bhqd', p_sel, v_sel)
                + jnp.einsum('bhqk,bhkd->bhqd', p[..., n_past:], v_own))

    out = lax.map(chunk, jnp.arange(seq_p // MOBA_Q_CHUNK))
    out = out.transpose(1, 0, 3, 2, 4).reshape(bsz, seq_p, n_heads, head_dim)
    return out[:, :seq]


def dilated_window_attention(q, k, v, dilation, steps):
    bsz, seq, n_heads, head_dim = q.shape
    sub = seq // dilation

    def to_residue(t):
        return t.reshape(bsz, sub, dilation, n_heads, head_dim).transpose(0, 2, 1, 3, 4).reshape(
            bsz * dilation, sub, n_heads, head_dim)

    def from_residue(t):
        tail = t.shape[2:]
        return t.reshape((bsz, dilation, sub) + tail).transpose((0, 2, 1) + tuple(range(3, 3 + len(tail)))).reshape(
            (bsz, seq) + tail)

    qr, kr, vr = to_residue(q), to_residue(k), to_residue(v)
    rows = bsz * dilation
    n_blk = -(-sub // steps)
    sub_p = n_blk * steps
    qb = jnp.pad(qr, ((0, 0), (0, sub_p - sub), (0, 0), (0, 0))).reshape(rows, n_blk, steps, n_heads, head_dim)
    kv_pad = ((0, 0), (steps, sub_p - sub), (0, 0), (0, 0))
    kp = jnp.pad(kr, kv_pad)
    vp = jnp.pad(vr, kv_pad)

    def band(t):
        return jnp.concatenate([t[:, :sub_p].reshape(rows, n_blk, steps, n_heads, head_dim),
                                t[:, steps:].reshape(rows, n_blk, steps, n_heads, head_dim)], axis=2)

    kb, vb = band(kp), band(vp)
    s = jnp.einsum('nbqhd,nbkhd->nbhqk', qb, kb).astype(jnp.float32) * (head_dim ** -0.5)
    qi = jnp.arange(steps)[:, None]
    kj = jnp.arange(2 * steps)[None, :]
    key_idx = jnp.arange(n_blk)[:, None, None] * steps - steps + kj
    mask = (kj >= qi) & (kj <= qi + steps) & (key_idx >= 0)
    s = jnp.where(mask[None, :, None], s, -jnp.inf)
    lse = jax.nn.logsumexp(s, axis=-1)
    p = jnp.exp(s - lse[..., None]).astype(v.dtype)
    o = jnp.einsum('nbhqk,nbkhd->nbqhd', p, vb).reshape(rows, sub_p, n_heads, head_dim)[:, :sub]
    lse = lse.transpose(0, 1, 3, 2).reshape(rows, sub_p, n_heads)[:, :sub]
    return from_residue(o), from_residue(lse)


def dilated_mixture(q, k, v):
    outs, lses = [], []
    for g, (window, dilation) in enumerate(DIL_PATTERNS):
        sl = slice(g * DIL_HEADS_PER_GROUP, (g + 1) * DIL_HEADS_PER_GROUP)
        o, l = dilated_window_attention(q[:, :, sl], k[:, :, sl], v[:, :, sl], dilation, window // dilation)
        outs.append(o)
        lses.append(l)
    w = jax.nn.softmax(jnp.stack(lses, axis=0), axis=0).astype(q.dtype)
    return jnp.sum(w[..., None] * jnp.stack(outs, axis=0), axis=0)


def mixer_block(h, w_in, w_proj_a, w_proj_b, w_out):
    bsz, seq, _ = h.shape
    proj = jnp.einsum('bsd,dc->bsc', h, w_in)
    sizes = [WIDTH_A] * 3 + [WIDTH_B] * 3 + [D_MODEL] * 2
    qa, ka, va, qb, kb, vb, ga, gb = jnp.split(proj, np.cumsum(sizes)[:-1].tolist(), axis=-1)
    pos = jnp.arange(seq)
    heads_a = (bsz, seq, MOBA_HEADS, HEAD_DIM)
    heads_b = (bsz, seq, N_DIL_GROUPS * DIL_HEADS_PER_GROUP, HEAD_DIM)
    oa = moba_attention(rope(qa.reshape(heads_a), pos), rope(ka.reshape(heads_a), pos), va.reshape(heads_a))
    ob = dilated_mixture(rope(qb.reshape(heads_b), pos), rope(kb.reshape(heads_b), pos), vb.reshape(heads_b))
    ya = jnp.einsum('bsc,cd->bsd', oa.reshape(bsz, seq, WIDTH_A), w_proj_a)
    yb = jnp.einsum('bsc,cd->bsd', ob.reshape(bsz, seq, WIDTH_B_OUT), w_proj_b)
    merged = jax.nn.sigmoid(ga) * ya + jax.nn.sigmoid(gb) * yb
    return jnp.einsum('bsd,de->bse', merged, w_out)


def swiglu(h, w_gate, w_up, w_down):
    a = jnp.einsum('bsd,df->bsf', h, w_gate)
    b = jnp.einsum('bsd,df->bsf', h, w_up)
    return jnp.einsum('bsf,fd->bsd', jax.nn.silu(a) * b, w_down)


def moe_swiglu(h, w_router, w_gate_e, w_up_e, w_down_e):
    logits = jnp.einsum('bsd,de->bse', h, w_router).astype(jnp.float32)
    top_val, top_idx = lax.top_k(logits, TOP_K)
    top_w = jax.nn.softmax(top_val, axis=-1)
    combine = jnp.sum(jax.nn.one_hot(top_idx, N_EXPERTS, dtype=jnp.float32) * top_w[..., None], axis=-2)
    combine = combine.astype(h.dtype)
    y = jnp.zeros_like(h)
    for e in range(N_EXPERTS):
        y = y + combine[..., e:e + 1] * swiglu(h, w_gate_e[e], w_up_e[e], w_down_e[e])
    return y


def setup_inputs(seed: int = 0) -> dict:
    key = jax.random.key(seed)
    keys = iter(jax.random.split(key, 32))

    def dense(shape, fan_in):
        return jax.random.normal(next(keys), shape, jnp.float32) * (fan_in ** -0.5)

    def gain():
        return 1.0 + 0.02 * jax.random.normal(next(keys), (D_MODEL,), jnp.float32)

    x = jax.random.normal(next(keys), (BATCH, SEQ, D_MODEL), jnp.float32)
    return {
        'x': x,
        'norm_mix_0': gain(),
        'w_in_0': dense((D_MODEL, IN_PROJ_COLS), D_MODEL),
        'w_proj_a_0': dense((WIDTH_A, D_MODEL), WIDTH_A),
        'w_proj_b_0': dense((WIDTH_B_OUT, D_MODEL), WIDTH_B_OUT),
        'w_out_0': dense((D_MODEL, D_MODEL), D_MODEL),
        'norm_ffn_0': gain(),
        'w_gate_0': dense((D_MODEL, D_FF_DENSE), D_MODEL),
        'w_up_0': dense((D_MODEL, D_FF_DENSE), D_MODEL),
        'w_down_0': dense((D_FF_DENSE, D_MODEL), D_FF_DENSE),
        'norm_mix_1': gain(),
        'w_in_1': dense((D_MODEL, IN_PROJ_COLS), D_MODEL),
        'w_proj_a_1': dense((WIDTH_A, D_MODEL), WIDTH_A),
        'w_proj_b_1': dense((WIDTH_B_OUT, D_MODEL), WIDTH_B_OUT),
        'w_out_1': dense((D_MODEL, D_MODEL), D_MODEL),
        'norm_ffn_1': gain(),
        'w_router_1': dense((D_MODEL, N_EXPERTS), D_MODEL),
        'w_gate_e_1': dense((N_EXPERTS, D_MODEL, D_FF_EXPERT), D_MODEL),
        'w_up_e_1': dense((N_EXPERTS, D_MODEL, D_FF_EXPERT), D_MODEL),
        'w_down_e_1': dense((N_EXPERTS, D_FF_EXPERT, D_MODEL), D_FF_EXPERT),
        'norm_final': gain(),
    }


def reference(x, norm_mix_0, w_in_0, w_proj_a_0, w_proj_b_0, w_out_0, norm_ffn_0, w_gate_0, w_up_0, w_down_0,
              norm_mix_1, w_in_1, w_proj_a_1, w_proj_b_1, w_out_1, norm_ffn_1, w_router_1, w_gate_e_1, w_up_e_1,
              w_down_e_1, norm_final):
    mixers = ((norm_mix_0, w_in_0, w_proj_a_0, w_proj_b_0, w_out_0),
              (norm_mix_1, w_in_1, w_proj_a_1, w_proj_b_1, w_out_1))
    ffns = ((norm_ffn_0, (w_gate_0, w_up_0, w_down_0)),
            (norm_ffn_1, (w_router_1, w_gate_e_1, w_up_e_1, w_down_e_1)))
    for layer in range(DEPTH):
        nm, wi, wa, wb, wo = mixers[layer]
        x = x + mixer_block(rms_norm(x, nm), wi, wa, wb, wo)
        nf, fp = ffns[layer]
        h = rms_norm(x, nf)
        if layer % 2 == 0:
            x = x + swiglu(h, *fp)
        else:
            x = x + moe_swiglu(h, *fp)
    return rms_norm(x, norm_final)
```

```python
import numpy as np
import ml_dtypes
import concourse.bass as bass
import concourse.mybir as mybir
from concourse.bass_utils import run_bass_kernel_spmd

F32 = mybir.dt.float32
BF16 = mybir.dt.bfloat16
ALU = mybir.AluOpType
AF = mybir.ActivationFunctionType
AX = mybir.AxisListType

D = 1024
S = 2048
NSEQ = 2
NCORES = 8
HD = 64
DFF0 = 2816
DFFE = 3584
NEXP = 8
INCOLS = 5888
NEG = -30000.0
EPS = 1e-6
NDSEM = 12


class Res:
    __slots__ = ("name", "w", "rs", "drs")

    def __init__(self, name=""):
        self.name = name
        self.w = None
        self.rs = {}
        self.drs = []


class Op:
    __slots__ = ("eng", "emit", "deps", "sig", "done", "isdma", "throttle", "bar")


class Prog:
    ENG = ("pe", "act", "dve", "pool", "sp")
    DMAQ = ("sp", "pool")

    def __init__(self):
        self.ops = {e: [] for e in self.ENG}
        self.last = {e: None for e in self.ENG}
        self.dmas = {q: [] for q in self.DMAQ}

    def add(self, eng, emit, R=(), W=(), dma=False):
        op = Op()
        op.eng = eng
        op.emit = emit
        op.sig = False
        op.done = None
        op.isdma = dma
        op.throttle = None
        op.bar = None
        deps = set()
        for r in R:
            if r.w is not None:
                deps.add(r.w)
        for w in W:
            if w.w is not None:
                deps.add(w.w)
            for q in w.rs.values():
                deps.add(q)
            for q in w.drs:
                deps.add(q)
        for r in R:
            if dma:
                r.drs.append(op)
            else:
                r.rs[eng] = op
        for w in W:
            w.w = op
            w.rs = {}
            w.drs = []
        op.deps = []
        for d in deps:
            if d is op:
                continue
            if (not d.isdma) and d.eng == "pe" and eng == "pe" and not dma:
                continue
            op.deps.append(d)
            if not d.isdma:
                d.sig = True
        self.ops[eng].append(op)
        if dma:
            self.dmas[eng].append(op)
        else:
            self.last[eng] = op
        return op

    def barrier(self):
        snap_ops = []
        for e in self.ENG:
            l = self.last[e]
            if l is not None:
                l.sig = True
                snap_ops.append(l)
        snap_dma = []
        for q in self.DMAQ:
            snap_dma.extend(self.dmas[q][-NDSEM:])
        for e in self.ENG:
            op = Op()
            op.eng = e
            op.emit = None
            op.sig = False
            op.done = None
            op.isdma = False
            op.throttle = None
            op.bar = True
            op.deps = [o for o in snap_ops if o.eng != e or e != "pe"] + list(snap_dma)
            self.ops[e].append(op)

    def finalize(self, nc, csem, dsem):
        for e in self.ENG:
            tick = 0
            k = {q: 0 for q in self.DMAQ}
            for op in self.ops[e]:
                if op.emit is None:
                    continue
                if op.isdma:
                    i = k[e]
                    k[e] += 1
                    sem = dsem[e][i % NDSEM]
                    op.done = (sem, 16 * (i // NDSEM + 1))
                    if i >= NDSEM:
                        op.throttle = (sem, 16 * (i // NDSEM))
                elif op.sig:
                    tick += 1
                    op.done = (csem[e], tick)

    def emit_engine(self, e, engine, final_waits=None):
        known = {}
        for op in self.ops[e]:
            need = {}
            for d in op.deps:
                sem, val = d.done
                key = id(sem)
                if need.get(key, (None, 0))[1] < val:
                    need[key] = (sem, val)
            if op.throttle is not None:
                sem, val = op.throttle
                key = id(sem)
                if need.get(key, (None, 0))[1] < val:
                    need[key] = (sem, val)
            for key, (sem, val) in need.items():
                if known.get(key, 0) < val:
                    engine.wait_ge(sem, val)
                    known[key] = val
            if op.emit is None:
                continue
            inst = op.emit(engine)
            if op.isdma:
                inst.then_inc(op.done[0], 16)
            elif op.sig:
                inst.then_inc(op.done[0], 1)
        if final_waits:
            for sem, val in final_waits:
                if known.get(id(sem), 0) < val:
                    engine.wait_ge(sem, val)


def host_consts():
    half = HD // 2
    inv = 10000.0 ** (-np.arange(half, dtype=np.float32) / half)
    pos = np.arange(S, dtype=np.float32)
    ang = pos[None, :] * inv[:, None]
    cos = np.cos(ang).astype(np.float32)
    sin = np.sin(ang).astype(np.float32)
    rope = np.zeros((2, 128, S), np.float32)
    for p in range(128):
        rope[0, p] = cos[p % 32]
        rope[1, p] = -sin[p % 32] if (p % 64) < 32 else sin[p % 32]
    j = np.arange(128)[:, None]
    i = np.arange(128)[None, :]
    ident = (j == i).astype(np.float32)
    mcur = np.where(j <= i, 0.0, NEG).astype(np.float32)
    mprev = np.where(j >= i, 0.0, NEG).astype(np.float32)
    ind = np.zeros((128, S), np.float32)
    for n in range(8):
        ind[n, n * 256:(n + 1) * 256] = 1.0
    pswap = (j == (i ^ 32)).astype(np.float32)
    cbf = np.concatenate([ident, mcur, mprev, ind, pswap], axis=1).astype(ml_dtypes.bfloat16)
    sel = np.zeros((128, 8, 128), np.float32)
    for e in range(8):
        sel[e, e, :] = 1.0
    vb = np.zeros((128, 8, 8), np.float32)
    keep = np.ones((128, 8, 8), np.float32)
    for qt in range(8):
        b = 4 + qt // 2
        vb[:, qt, b:] = -1e30
        keep[:, qt, b] = 0.0
    cf32 = np.concatenate([ident, sel.reshape(128, 1024), vb.reshape(128, 64), keep.reshape(128, 64)], axis=1)
    return rope, cbf, cf32.astype(np.float32)


def build(nseq=NSEQ, stop_after=None, dbg=(), dbg_scratch=False):
    nc = bass.Bass("TRN2", target_bir_lowering=False)
    P = Prog()

    def din(name, shape, dt=F32):
        return nc.dram_tensor(name, list(shape), dt, kind="ExternalInput").ap()

    x_in = din("x", [nseq, S, D])
    W = {}
    for l in range(2):
        W[f"norm_mix_{l}"] = din(f"norm_mix_{l}", [D])
        W[f"w_in_{l}"] = din(f"w_in_{l}", [D, INCOLS])
        W[f"w_proj_a_{l}"] = din(f"w_proj_a_{l}", [512, D])
        W[f"w_proj_b_{l}"] = din(f"w_proj_b_{l}", [256, D])
        W[f"w_out_{l}"] = din(f"w_out_{l}", [D, D])
        W[f"norm_ffn_{l}"] = din(f"norm_ffn_{l}", [D])
    W["w_gate_0"] = din("w_gate_0", [D, DFF0])
    W["w_up_0"] = din("w_up_0", [D, DFF0])
    W["w_down_0"] = din("w_down_0", [DFF0, D])
    W["w_router_1"] = din("w_router_1", [D, NEXP])
    W["w_gate_e_1"] = din("w_gate_e_1", [NEXP, D, DFFE])
    W["w_up_e_1"] = din("w_up_e_1", [NEXP, D, DFFE])
    W["w_down_e_1"] = din("w_down_e_1", [NEXP, DFFE, D])
    W["norm_final"] = din("norm_final", [D])
    c_rope = din("c_rope", [2, 128, S])
    c_bf = din("c_bf", [128, 512 + S], BF16)
    c_f32 = din("c_f32", [128, 128 + 1024 + 64 + 64])
    y_out = nc.dram_tensor("y", [nseq, S, D], F32, kind="ExternalOutput").ap()
    dbg_out = {}
    for name in dbg:
        dbg_out[name] = nc.dram_tensor("dbg_" + name, [nseq, 8, 128, S], F32, kind="ExternalOutput").ap()

    def dscr(name, shape, dt=BF16):
        return nc.dram_tensor(name, list(shape), dt, kind="ExternalOutput" if dbg_scratch else "Internal").ap()

    qk_d = dscr("qk_d", [nseq, 20, 128, S])
    v_d = dscr("v_d", [nseq, S, 20, 128])
    sg_d = dscr("sg_d", [nseq, 16, 128, S])
    o_d = dscr("o_d", [nseq, 6, 128, S])
    qk_res = [[Res(f"qkd{s}_{f}") for f in range(20)] for s in range(nseq)]
    v_res = [[Res(f"vd{s}_{t}_{g}") for t in range(16) for g in range(3)] for s in range(nseq)]
    sg_res = [[Res(f"sgd{s}_{f}") for f in range(16)] for s in range(nseq)]
    o_res = [[Res(f"od{s}_{f}") for f in range(6)] for s in range(nseq)]

    xres = nc.alloc_sbuf_tensor("xres", [128, 8, S], F32)
    xres_r = [[Res(f"x{fc}_{tc}") for tc in range(4)] for fc in range(8)]
    hT = nc.alloc_sbuf_tensor("hT", [128, 8, S], BF16)
    hT_r = [[Res(f"h{fc}_{tc}") for tc in range(4)] for fc in range(8)]
    cbf = nc.alloc_sbuf_tensor("cbf", [128, 512 + S], BF16)
    cbf_r = Res("cbf")
    cf = nc.alloc_sbuf_tensor("cf", [128, 128 + 1024 + 64 + 64], F32)
    cf_r = Res("cf")
    gcols = nc.alloc_sbuf_tensor("gcols", [128, 5, 8], F32)
    gcols_r = Res("gcols")
    onesb = nc.alloc_sbuf_tensor("onesb", [128, 128], BF16)
    onesb_r = Res("onesb")
    epsc = nc.alloc_sbuf_tensor("epsc", [128, 8], F32)
    epsc_r = Res("epsc")
    ARENA = 100 * 1024 // 2
    arena = nc.alloc_sbuf_tensor("arena", [128, ARENA], BF16)
    ps = nc.alloc_psum_tensor("ps", [128, 8, 512], F32)
    PB = [Res(f"bank{i}") for i in range(8)]

    ident_bf = cbf[:, 0:128]
    mcur = cbf[:, 128:256]
    mcurprev = cbf[:, 128:384]
    ind = cbf[:, 384:384 + S]
    pswap = cbf[:, 384 + S:512 + S]
    ident32 = cf[:, 0:128]
    sel = cf[:, 128:1152].rearrange("p (e n) -> p e n", e=8)
    vbias = cf[:, 1152:1216]
    keepm = cf[:, 1216:1280]

    class Arena:
        def __init__(self):
            self.off = 0

        def reset(self):
            P.barrier()
            self.off = 0

        def tile(self, cols, dt, name=""):
            n0 = cols * (2 if dt == F32 else 1)
            n = (n0 + 31) // 32 * 32
            assert self.off + n <= ARENA, ("arena overflow", name, self.off, n)
            a = arena[:, self.off:self.off + n0]
            self.off += n
            if dt == F32:
                a = a.bitcast(F32)
            return a

    A = Arena()
    bank_ctr = [0]

    def nbank():
        b = bank_ctr[0] % 8
        bank_ctr[0] += 1
        return b

    def dma(q, out, in_, R, Wr, **kw):
        return P.add(q, lambda e: e.dma_start(out=out, in_=in_, **kw), R=R, W=Wr, dma=True)

    def mm(out, lhsT, rhs, start, stop, R, Wr, **kw):
        return P.add("pe", lambda e: e.matmul(out, lhsT, rhs, start=start, stop=stop, **kw), R=R, W=Wr)

    def act(out, in_, func, R, Wr, scale=1.0, bias=0.0):
        return P.add("act", lambda e: e.activation(out=out, in_=in_, func=func, bias=bias, scale=scale), R=R, W=Wr)

    def tt(eng, out, in0, in1, op, R, Wr):
        return P.add(eng, lambda e: e.tensor_tensor(out=out, in0=in0, in1=in1, op=op), R=R, W=Wr)

    def cp(eng, out, in_, R, Wr):
        if eng == "act":
            return P.add("act", lambda e: e.copy(out=out, in_=in_), R=R, W=Wr)
        return P.add(eng, lambda e: e.tensor_copy(out=out, in_=in_), R=R, W=Wr)

    dma("sp", cbf[:], c_bf[:, :], [], [cbf_r])
    dma("sp", cf[:], c_f32[:, :], [], [cf_r])
    gnames = ["norm_mix_0", "norm_ffn_0", "norm_mix_1", "norm_ffn_1", "norm_final"]
    for i, gn in enumerate(gnames):
        dma("sp", gcols[:, i, :], W[gn].rearrange("(c p) -> p c", p=128), [], [gcols_r],
            allow_slow_non_contiguous=True)
    P.add("pool", lambda e: e.memset(onesb[:], 1.0), W=[onesb_r])
    P.add("pool", lambda e: e.memset(epsc[:], EPS), W=[epsc_r])

    def load_x(s):
        A.reset()
        xin = [A.tile(D, F32, "xin") for _ in range(4)]
        xin_r = [Res(f"xin{i}") for i in range(4)]
        for t in range(16):
            b = t % 4
            dma("sp", xin[b], x_in[s, t * 128:(t + 1) * 128, :], [], [xin_r[b]])
            for half in range(2):
                bk = nbank()
                for c in range(4):
                    fc = half * 4 + c
                    P.add("pe", lambda e, bk=bk, c=c, fc=fc, b=b: e.transpose(
                        ps[:, bk, c * 128:(c + 1) * 128], xin[b][:, fc * 128:(fc + 1) * 128], ident32),
                        R=[xin_r[b], cf_r], W=[PB[bk]])
                tcx = t // 4
                outv = xres[:, half * 4:half * 4 + 4, t * 128:(t + 1) * 128]
                inv = ps[:, bk, :].rearrange("p (c n) -> p c n", c=4)
                cp("act" if half == 0 else "dve", outv, inv, [PB[bk]], [xres_r[half * 4 + c][tcx] for c in range(4)])

    def norm(gi, router=None, reset=True):
        if reset:
            A.reset()
        sqb = [A.tile(8 * 512, BF16, "sqb").rearrange("p (c n) -> p c n", c=8) for _ in range(2)]
        sqb_r = [Res("sq0"), Res("sq1")]
        rs = [A.tile(512, F32, "rs") for _ in range(2)]
        rs_r = [Res("rs0"), Res("rs1")]
        rstd = [A.tile(512, F32, "rstd") for _ in range(2)]
        rstd_r = [Res("rstd0"), Res("rstd1")]
        for tc in range(4):
            b = tc % 2
            ch = slice(tc * 512, (tc + 1) * 512)
            act(sqb[b][:], xres[:, :, ch], AF.Square, [xres_r[fc][tc] for fc in range(8)], [sqb_r[b]])
            bk = nbank()
            for fc in range(8):
                mm(ps[:, bk, :], onesb[:], sqb[b][:, fc, :], fc == 0, fc == 7, [onesb_r, sqb_r[b]], [PB[bk]])
            act(rs[b], ps[:, bk, :], AF.Ln, [PB[bk], epsc_r], [rs_r[b]], scale=1.0 / D, bias=epsc[:, 0:1])
            act(rstd[b], rs[b], AF.Exp, [rs_r[b]], [rstd_r[b]], scale=-0.5)
            for fc in range(8):
                if router is None:
                    P.add("dve", lambda e, fc=fc, ch=ch, b=b: e.scalar_tensor_tensor(
                        out=hT[:, fc, ch], in0=xres[:, fc, ch], scalar=gcols[:, gi, fc:fc + 1], in1=rstd[b],
                        op0=ALU.mult, op1=ALU.mult),
                        R=[xres_r[fc][tc], gcols_r, rstd_r[b]], W=[hT_r[fc][tc]])
                else:
                    router["emit_h32"](tc, fc, ch, b, rstd, rstd_r, gi)
            if router is not None:
                router["after_chunk"](tc)

    def final_out(s):
        A.reset()
        sqb = [A.tile(8 * 512, BF16, "sqb").rearrange("p (c n) -> p c n", c=8) for _ in range(2)]
        sqb_r = [Res("sq0"), Res("sq1")]
        rs = [A.tile(512, F32, "rs") for _ in range(2)]
        rs_r = [Res("rs0"), Res("rs1")]
        rstd = [A.tile(512, F32, "rstd") for _ in range(2)]
        rstd_r = [Res("rstd0"), Res("rstd1")]
        y32 = [A.tile(8 * 512, F32, "y32").rearrange("p (c n) -> p c n", c=8) for _ in range(2)]
        y32_r = [Res("y0"), Res("y1")]
        ost = [A.tile(D, F32, "ost") for _ in range(4)]
        ost_r = [Res(f"ost{i}") for i in range(4)]
        k = 0
        for tc in range(4):
            b = tc % 2
            ch = slice(tc * 512, (tc + 1) * 512)
            act(sqb[b][:], xres[:, :, ch], AF.Square, [xres_r[fc][tc] for fc in range(8)], [sqb_r[b]])
            bk = nbank()
            for fc in range(8):
                mm(ps[:, bk, :], onesb[:], sqb[b][:, fc, :], fc == 0, fc == 7, [onesb_r, sqb_r[b]], [PB[bk]])
            act(rs[b], ps[:, bk, :], AF.Ln, [PB[bk], epsc_r], [rs_r[b]], scale=1.0 / D, bias=epsc[:, 0:1])
            act(rstd[b], rs[b], AF.Exp, [rs_r[b]], [rstd_r[b]], scale=-0.5)
            for fc in range(8):
                P.add("dve", lambda e, fc=fc, ch=ch, b=b: e.scalar_tensor_tensor(
                    out=y32[b][:, fc, :], in0=xres[:, fc, ch], scalar=gcols[:, 4, fc:fc + 1], in1=rstd[b],
                    op0=ALU.mult, op1=ALU.mult),
                    R=[xres_r[fc][tc], gcols_r, rstd_r[b]], W=[y32_r[b]])
            for tl in range(4):
                ob = k % 4
                k += 1
                t = tc * 4 + tl
                for half in range(2):
                    bk = nbank()
                    for c in range(4):
                        fc = half * 4 + c
                        P.add("pe", lambda e, bk=bk, c=c, fc=fc, b=b, tl=tl: e.transpose(
                            ps[:, bk, c * 128:(c + 1) * 128], y32[b][:, fc, tl * 128:(tl + 1) * 128], ident32),
                            R=[y32_r[b], cf_r], W=[PB[bk]])
                    cp("act" if half == 0 else "dve", ost[ob][:, half * 512:(half + 1) * 512], ps[:, bk, :],
                       [PB[bk]], [ost_r[ob]])
                dma("sp", y_out[s, t * 128:(t + 1) * 128, :], ost[ob], [ost_r[ob]], [])

    def dump_x(s, name):
        if name in dbg_out:
            for fc in range(8):
                dma("sp", dbg_out[name][s, fc, :, :], xres[:, fc, :], [xres_r[fc][tc] for tc in range(4)], [])

    def in_proj(s, l):
        A.reset()
        w_in = W[f"w_in_{l}"]
        rope_t = A.tile(2 * S, F32, "rope").rearrange("p (c n) -> p c n", c=2)
        rope_r = Res("rope")
        dma("sp", rope_t[:, 0, :], c_rope[0, :, :], [], [rope_r])
        dma("sp", rope_t[:, 1, :], c_rope[1, :, :], [], [rope_r])
        wsl = [A.tile(8 * 512, BF16, "wsl").rearrange("p (c n) -> p c n", c=8) for _ in range(2)]
        wsl_r = [Res("wsl0"), Res("wsl1")]
        stage = [A.tile(S, BF16, "stage") for _ in range(2)]
        stage_r = [Res("st0"), Res("st1")]
        xb = [A.tile(512, BF16, "xb") for _ in range(4)]
        xs_r = [Res(f"xs{i}") for i in range(4)]
        pend = []
        t1 = [A.tile(512, F32, "t1") for _ in range(4)]
        t1_r = [Res(f"t1{i}") for i in range(4)]
        t2 = [A.tile(512, F32, "t2") for _ in range(4)]
        t2_r = [[Res(f"t2{i}{q}") for q in range(4)] for i in range(4)]
        vst = [A.tile(8 * 128, BF16, "vst").rearrange("p (h c) -> p h c", h=8) for _ in range(2)]
        vst_r = [Res("vst0"), Res("vst1")]
        for b in range(2):
            P.add("pool", lambda e, b=b: e.memset(vst[b][:], 1.0), W=[vst_r[b]])
        slabs = [(0, 512, "qk", 0), (512, 512, "qk", 4), (1024, 512, "v", 0),
                 (1536, 512, "qk", 8), (2048, 512, "qk", 12), (2560, 512, "qk", 16),
                 (3072, 512, "v", 8), (3584, 256, "v", 16),
                 (3840, 512, "g", 0), (4352, 512, "g", 4), (4864, 512, "g", 8), (5376, 512, "g", 12)]

        def perm_d(ft):
            if ft in (10, 11, 16, 17):
                return 4
            if ft in (12, 13, 18, 19):
                return 16
            return 1
        cnt = {"xs": 0, "t": 0, "st": 0, "vst": 0}

        def load_slab(i):
            c0, w, kind, base = slabs[i]
            b = i % 2
            dma("pool", wsl[b][:, :, 0:w], w_in[:, c0:c0 + w].rearrange("(c p) n -> p c n", p=128),
                [], [wsl_r[b]])
        load_slab(0)
        norm(0 if l == 0 else 2, reset=False)
        for i, (c0, w, kind, base) in enumerate(slabs):
            b = i % 2
            if i + 1 < len(slabs):
                load_slab(i + 1)
            if kind in ("qk", "g"):
                for fl in range(w // 128):
                    ft = base + fl
                    sb = cnt["st"] % 2
                    cnt["st"] += 1
                    for tc in range(4):
                        ch = slice(tc * 512, (tc + 1) * 512)
                        bk = nbank()
                        for kc in range(8):
                            mm(ps[:, bk, :], wsl[b][:, kc, fl * 128:(fl + 1) * 128], hT[:, kc, ch], kc == 0, kc == 7,
                               [wsl_r[b], hT_r[kc][tc]], [PB[bk]])
                        if kind == "g":
                            act(stage[sb][:, ch], ps[:, bk, :], AF.Sigmoid, [PB[bk]], [stage_r[sb]])
                        else:
                            xi = cnt["xs"] % 4
                            cnt["xs"] += 1
                            ti = cnt["t"] % 4
                            cnt["t"] += 1
                            cp("act", xb[xi], ps[:, bk, :], [PB[bk]], [xs_r[xi]])
                            tt("dve", t1[ti], ps[:, bk, :], rope_t[:, 0, ch], ALU.mult, [PB[bk], rope_r, xs_r[xi]], [t1_r[ti]])
                            d = perm_d(ft)
                            if d == 1:
                                outv = stage[sb][:, ch]
                                in0, in1 = t1[ti], t2[ti]
                            else:
                                n = 512 // d
                                outv = stage[sb].rearrange("p (r m) -> p r m", r=d)[:, :, tc * n:(tc + 1) * n]
                                in0 = t1[ti].rearrange("p (m r) -> p r m", r=d)
                                in1 = t2[ti].rearrange("p (m r) -> p r m", r=d)

                            def finish(xi=xi, ti=ti, ch=ch, outv=outv, in0=in0, in1=in1, sb=sb):
                                b2 = nbank()
                                mm(ps[:, b2, :], pswap, xb[xi], True, True, [cbf_r, xs_r[xi]], [PB[b2]])
                                tt("dve", t2[ti], ps[:, b2, :], rope_t[:, 1, ch], ALU.mult, [PB[b2], rope_r], [t2_r[ti][0]])
                                tt("pool", outv, in0, in1, ALU.add, [t1_r[ti], t2_r[ti][0]], [stage_r[sb]])
                            while pend:
                                pend.pop(0)()
                            pend.append(finish)
                            if tc == 3:
                                pend.append(lambda s_=s, ft=ft, sb=sb: dma("sp", qk_d[s_, ft, :, :], stage[sb],
                                                                           [stage_r[sb]], [qk_res[s_][ft]]))
                    if kind == "g":
                        dma("sp", sg_d[s, ft, :, :], stage[sb], [stage_r[sb]], [sg_res[s][ft]])
                while pend:
                    pend.pop(0)()
            else:
                nh = w // 64
                for t in range(16):
                    bk = nbank()
                    for kc in range(8):
                        mm(ps[:, bk, 0:w], hT[:, kc, t * 128:(t + 1) * 128], wsl[b][:, kc, 0:w], kc == 0, kc == 7,
                           [wsl_r[b], hT_r[kc][t // 4]], [PB[bk]])
                    vb_ = cnt["vst"] % 2
                    cnt["vst"] += 1
                    cp("act" if t % 2 == 0 else "dve", vst[vb_][:, 0:nh, 0:64],
                       ps[:, bk, 0:w].rearrange("p (h c) -> p h c", c=64), [PB[bk]], [vst_r[vb_]])
                    grp = 0 if base == 0 else (1 if base == 8 else 2)
                    dma("sp", v_d[s, t * 128:(t + 1) * 128, base:base + nh, :], vst[vb_][:, 0:nh, :],
                        [vst_r[vb_]], [v_res[s][t * 3 + grp]])

    pw = {}
    hflat = hT[:].rearrange("p c n -> p (c n)")

    def prefetch_proj_weights(l):
        pw["wpa"] = hflat[:, 0:4 * D].rearrange("p (c n) -> p c n", c=4)
        pw["wpb"] = hflat[:, 4 * D:6 * D].rearrange("p (c n) -> p c n", c=2)
        pw["wout"] = hflat[:, 6 * D:14 * D].rearrange("p (c n) -> p c n", c=8)
        pw["wpa_r"], pw["wpb_r"], pw["wout_r"] = Res("wpa"), Res("wpb"), Res("wout")
        allh = [hT_r[fc][tc] for fc in range(8) for tc in range(4)]
        dma("pool", pw["wpa"], W[f"w_proj_a_{l}"].rearrange("(c p) n -> p c n", p=128), [], [pw["wpa_r"]] + allh)
        dma("pool", pw["wpb"], W[f"w_proj_b_{l}"].rearrange("(c p) n -> p c n", p=128), [], [pw["wpb_r"]] + allh)
        for hlf in range(2):
            dma("pool", pw["wout"][:, hlf * 4:(hlf + 1) * 4, :],
                W[f"w_out_{l}"][hlf * 512:(hlf + 1) * 512, :].rearrange("(c p) n -> p c n", p=128), [],
                [pw["wout_r"]] + allh)

    def attention(s, l):
        A.reset()
        prefetch_proj_weights(l)
        SC = HD ** -0.5
        kst = [A.tile(S, BF16, "kst") for _ in range(2)]
        qA = [A.tile(S, BF16, "qA") for _ in range(2)]
        qB = [A.tile(S, BF16, "qB") for _ in range(2)]
        Vt = [A.tile(16 * 256, BF16, "Vt").rearrange("p (t h c) -> p t h c", t=16, h=2) for _ in range(2)]
        kst_r = [Res("kst0"), Res("kst1")]
        qA_r = [Res("qA0"), Res("qA1")]
        qB_r = [Res("qB0"), Res("qB1")]
        Vt_r = [Res("Vt0"), Res("Vt1")]
        for b in range(2):
            P.add("pool", lambda e, b=b: e.memset(qA[b][64:128, :], 0.0), W=[qA_r[b]])
            P.add("pool", lambda e, b=b: e.memset(qB[b][0:64, :], 0.0), W=[qB_r[b]])
        Uacc = A.tile(2 * S, F32, "Uacc").rearrange("p (h n) -> p h n", h=2)
        Uacc_r = [[Res(f"U{h}_{t}") for t in range(16)] for h in range(2)]
        pT = [A.tile(512, BF16, "pT") for _ in range(6)]
        pT_r = [Res(f"pT{i}") for i in range(6)]
        ostg = [A.tile(S, BF16, "ostg") for _ in range(2)]
        ostg_r = [Res("og0"), Res("og1")]
        rden = [A.tile(512, F32, "rden") for _ in range(2)]
        rden_r = [Res("rd0"), Res("rd1")]
        rdscr = [A.tile(512, F32, "rdscr") for _ in range(2)]
        rdscr_r = [Res("rds0"), Res("rds1")]
        usb = [A.tile(512, F32, "usb") for _ in range(2)]
        usb_r = [Res("us0"), Res("us1")]
        ks32 = [A.tile(8, F32, "ks32") for _ in range(2)]
        ks32_r = [Res("ks320"), Res("ks321")]
        ksb = [A.tile(8, BF16, "ksb") for _ in range(2)]
        ksb_r = [Res("ksb0"), Res("ksb1")]
        gv = [A.tile(64, F32, "gv") for _ in range(2)]
        gv_r = [Res("gv0"), Res("gv1")]
        top8 = [A.tile(64, F32, "top8") for _ in range(2)]
        top8_r = [Res("top80"), Res("top81")]
        mbf = [A.tile(64, F32, "mbf") for _ in range(2)]
        mbf_r = [Res("mbf0"), Res("mbf1")]
        mbb = [A.tile(8 * 128, BF16, "mbb").rearrange("p (q n) -> p q n", q=8) for _ in range(2)]
        mbb_r = [Res("mbb0"), Res("mbb1")]
        for i in range(2):
            P.add("pool", lambda e, i=i: e.memset(mbb[i][:], 0.0), W=[mbb_r[i]])
        mbT = [A.tile(1024, BF16, "mbT") for _ in range(2)]
        mbT_r = [Res("mbT0"), Res("mbT1")]
        cnt = {"nrm": 0, "og": 0, "bank0": 0}
        SB = [0, 1, 7, 4]
        OB = [2, 3]

        units = []
        for u in range(4):
            units.append(("moba", u, 4 + u, 2 * u, 1, u, 0))
        for pair in range(2):
            for g in range(3):
                units.append(("dil", 8 + 2 * g + pair, 14 + 2 * g + pair, 8 + 4 * g + 2 * pair, (1, 4, 16)[g], 4 + pair, g))

        def load_unit(ui):
            kind, qft, kft, h0, d, oft, g = units[ui]
            b = ui % 2
            dma("sp", kst[b], qk_d[s, kft, :, :], [qk_res[s][kft]], [kst_r[b]])
            dma("sp", qA[b][0:64, :], qk_d[s, qft, 0:64, :], [qk_res[s][qft]], [qA_r[b]])
            dma("sp", qB[b][64:128, :], qk_d[s, qft, 64:128, :], [qk_res[s][qft]], [qB_r[b]])
            vres = [v_res[s][t * 3 + (0 if h0 < 8 else (1 if h0 < 16 else 2))] for t in range(16)]
            if d == 1:
                src = v_d[s, :, h0:h0 + 2, :].rearrange("(t j) h c -> j t h c", j=128)
                dma("sp", Vt[b][:], src, vres, [Vt_r[b]])
            else:
                nt = 16 // d
                for r in range(d):
                    src = v_d[s, :, h0:h0 + 2, :].rearrange("(m j r) h c -> r j m h c", j=128, r=d)[r]
                    dma("sp", Vt[b][:, r * nt:(r + 1) * nt, :, :], src, vres, [Vt_r[b]])
            if kind == "moba":
                P.add("dve", lambda e, b=b: e.tensor_reduce(out=ks32[b], in_=kst[b].rearrange("p (n k) -> p n k", n=8),
                                                            axis=AX.X, op=ALU.add), R=[kst_r[b]], W=[ks32_r[b]])
                cp("dve", ksb[b], ks32[b], [ks32_r[b]], [ksb_r[b]])

        def normalize_store(src_of, srcR, hp, og, qc, use_act=False):
            i = cnt["nrm"] % 2
            cnt["nrm"] += 1
            ch = slice(qc * 512, (qc + 1) * 512)
            if use_act:
                act(rdscr[i][64:128, :], src_of(64, 128), AF.Ln, srcR, [rdscr_r[i]])
                act(rden[i][64:128, :], rdscr[i][64:128, :], AF.Exp, [rdscr_r[i]], [rden_r[i]], scale=-1.0)
            else:
                P.add("dve", lambda e: e.reciprocal(out=rden[i][64:128, :], in_=src_of(64, 128)), R=srcR, W=[rden_r[i]])
            cp("act", usb[i][64:128, :], src_of(0, 64), list(srcR) + [rden_r[i]], [usb_r[i]])
            tt("dve", ostg[og][hp * 64:(hp + 1) * 64, ch], usb[i][64:128, :], rden[i][64:128, :], ALU.mult,
               [usb_r[i], rden_r[i]], [ostg_r[og]])

        stages = []

        def gating_part1(b, hp, hi):
            Q = qA[b] if hp == 0 else qB[b]
            Q_r = qA_r[b] if hp == 0 else qB_r[b]
            for j in range(8):
                qt = 8 + j
                mm(ps[:, 4, j * 8:(j + 1) * 8], Q[:, qt * 128:(qt + 1) * 128], ksb[b], True, True,
                   [Q_r, ksb_r[b]], [PB[4]])
            tt("dve", gv[hi], ps[:, 4, 0:64], vbias, ALU.add, [PB[4], cf_r], [gv_r[hi]])
            for j in range(8):
                P.add("dve", lambda e, j=j: e.max(out=top8[hi][:, j * 8:(j + 1) * 8], in_=gv[hi][:, j * 8:(j + 1) * 8]),
                      R=[gv_r[hi]], W=[top8_r[hi]])
            for j in range(8):
                P.add("dve", lambda e, j=j: e.tensor_scalar(
                    out=mbf[hi][:, j * 8:(j + 1) * 8], in0=gv[hi][:, j * 8:(j + 1) * 8],
                    scalar1=top8[hi][:, j * 8 + 2:j * 8 + 3], scalar2=NEG, op0=ALU.is_lt, op1=ALU.mult),
                    R=[gv_r[hi], top8_r[hi]], W=[mbf_r[hi]])
            tt("dve", mbb[hi][:, :, 0:8], mbf[hi].rearrange("p (q n) -> p q n", q=8),
               keepm.rearrange("p (q n) -> p q n", q=8), ALU.mult, [mbf_r[hi], cf_r], [mbb_r[hi]])

        def gating_part2(hi):
            for j in range(8):
                bk = 5 + j // 4
                mm(ps[:, bk, (j % 4) * 128:(j % 4 + 1) * 128], mbb[hi][:, j, :], ident_bf, True, True,
                   [mbb_r[hi], cbf_r], [PB[bk]])
            cp("act", mbT[hi][:, 0:512], ps[:, 5, :], [PB[5]], [mbT_r[hi]])
            cp("act", mbT[hi][:, 512:1024], ps[:, 6, :], [PB[6]], [mbT_r[hi]])

        def moba_stage(b, hp, hi, qc, kt, nk, og, oft, last_of_unit):
            si = len(stages)
            sbk = SB[si % 4]
            pi = si % 6
            Q = qA[b] if hp == 0 else qB[b]
            Q_r = qA_r[b] if hp == 0 else qB_r[b]
            K = kst[b]
            if kt < 4 * qc:
                off, N = 0, 512
            else:
                off = (kt - 4 * qc) * 128
                N = 512 - off
            q0 = qc * 512 + off
            diag = kt >= 4 * qc
            gate = qc >= 2
            ob = OB[qc % 2]

            def qk():
                mm(ps[:, sbk, off:off + N], K[:, kt * 128:(kt + 1) * 128], Q[:, q0:q0 + N], True,
                   not (diag or gate), [kst_r[b], Q_r], [PB[sbk]])
                if gate:
                    mm(ps[:, sbk, off:off + N], ind[:, kt * 128:(kt + 1) * 128],
                       mbT[hi][:, q0 - 1024:q0 - 1024 + N], False, not diag, [cbf_r, mbT_r[hi]], [PB[sbk]])
                if diag:
                    mm(ps[:, sbk, off:off + 128], ident_bf, mcur, False, True, [cbf_r], [PB[sbk]])
                act(pT[pi][:, off:off + N], ps[:, sbk, off:off + N], AF.Exp, [PB[sbk]], [pT_r[pi]], scale=SC)

            def pv():
                mm(ps[:, ob, off:off + N], Vt[b][:, kt, hp, :], pT[pi][:, off:off + N], kt == 0,
                   kt == nk - 1, [Vt_r[b], pT_r[pi]], [PB[ob]])
                if kt == nk - 1:
                    normalize_store(lambda lo, hi_, ob=ob: ps[lo:hi_, ob, :], [PB[ob]], hp, og, qc)
                    if last_of_unit:
                        dma("sp", o_d[s, oft, :, :], ostg[og], [ostg_r[og]], [o_res[s][oft]])
            return qk, pv

        def dil_stage(b, hp, d, g, r, m, nt, bank0, og, oft, last_of_unit):
            si = len(stages)
            sbk = SB[si % 4]
            pi = si % 6
            Q = qA[b] if hp == 0 else qB[b]
            Q_r = qA_r[b] if hp == 0 else qB_r[b]
            K = kst[b]
            Uh = Uacc[:, hp, :]
            T = r * nt + m
            N = 256 if m + 1 < nt else 128
            ob1 = OB[(bank0 + T // 4) % 2]

            def qk():
                mm(ps[:, sbk, 0:N], K[:, T * 128:(T + 1) * 128], Q[:, T * 128:T * 128 + N], True, False,
                   [kst_r[b], Q_r], [PB[sbk]])
                mm(ps[:, sbk, 0:N], ident_bf, mcurprev[:, 0:N], False, True, [cbf_r], [PB[sbk]])
                act(pT[pi][:, 0:N], ps[:, sbk, 0:N], AF.Exp, [PB[sbk]], [pT_r[pi]], scale=SC)

            def pv():
                mm(ps[:, ob1, (T % 4) * 128:(T % 4 + 1) * 128], Vt[b][:, T, hp, :], pT[pi][:, 0:128],
                   m == 0, True, [Vt_r[b], pT_r[pi]], [PB[ob1]], skip_group_check=True)
                if N == 256:
                    T2 = T + 1
                    ob2 = OB[(bank0 + T2 // 4) % 2]
                    mm(ps[:, ob2, (T2 % 4) * 128:(T2 % 4 + 1) * 128], Vt[b][:, T, hp, :],
                       pT[pi][:, 128:256], True, False, [Vt_r[b], pT_r[pi]], [PB[ob2]], skip_group_check=True)
                if T % 4 == 3:
                    kb4 = T // 4
                    src = ps[:, ob1, :]
                    if d == 1:
                        dst = Uh[:, kb4 * 512:(kb4 + 1) * 512]
                        dres = [Uacc_r[hp][kb4 * 4 + i] for i in range(4)]
                    elif d == 4:
                        dst = Uh.rearrange("p (m j r) -> p r m j", j=128, r=4)[:, kb4, :, :]
                        src = src.rearrange("p (m j) -> p m j", m=4)
                        dres = Uacc_r[hp]
                    else:
                        dst = Uh.rearrange("p (j r) -> p r j", r=16)[:, kb4 * 4:(kb4 + 1) * 4, :]
                        src = src.rearrange("p (m j) -> p m j", m=4)
                        dres = Uacc_r[hp]
                    if g == 0:
                        cp("act" if kb4 % 2 == 0 else "dve", dst, src, [PB[ob1]], dres)
                    else:
                        tt("dve", dst, src, dst, ALU.add, [PB[ob1]] + dres, dres)
                if last_of_unit and g == 2:
                    for hp2 in range(2):
                        for qc in range(4):
                            normalize_store(lambda lo, hi_, hp2=hp2, qc=qc: Uacc[lo:hi_, hp2, qc * 512:(qc + 1) * 512],
                                            [Uacc_r[hp2][qc * 4 + i] for i in range(4)], hp2, og, qc, use_act=True)
                    dma("sp", o_d[s, oft, :, :], ostg[og], [ostg_r[og]], [o_res[s][oft]])
            return qk, pv

        hcount = 0
        for ui, (kind, qft, kft, h0, d, oft, g) in enumerate(units):
            b = ui % 2
            first_pre = []
            if ui + 1 < len(units):
                first_pre.append(lambda ui=ui: load_unit(ui + 1))
            if kind == "moba":
                og = cnt["og"] % 2
                cnt["og"] += 1
                for hp in range(2):
                    hi = hcount % 2
                    hcount += 1
                    tl = [(qc, kt, 4 * qc + 4) for qc in range(4) for kt in range(4 * qc + 4)]
                    for ti, (qc, kt, nk) in enumerate(tl):
                        pre = []
                        if ti == 3 and hp == 0:
                            pre += first_pre
                        if ti == 0:
                            pre.append(lambda b=b, hp=hp, hi=hi: gating_part1(b, hp, hi))
                        if qc == 2 and kt == 0:
                            pre.append(lambda hi=hi: gating_part2(hi))
                        last = (hp == 1 and ti == len(tl) - 1)
                        qk, pv = moba_stage(b, hp, hi, qc, kt, nk, og, oft, last)
                        stages.append((pre, qk, pv))
            else:
                nt = 16 // d
                if g == 2:
                    og = cnt["og"] % 2
                    cnt["og"] += 1
                else:
                    og = None
                for hp in range(2):
                    bank0 = cnt["bank0"]
                    cnt["bank0"] += 4
                    for r in range(d):
                        for m in range(nt):
                            pre = []
                            if hp == 0 and r * nt + m == 3:
                                pre += first_pre
                            last = (hp == 1 and r == d - 1 and m == nt - 1)
                            qk, pv = dil_stage(b, hp, d, g, r, m, nt, bank0, og, oft, last)
                            stages.append((pre, qk, pv))
        load_unit(0)
        SKEW = 3
        for i in range(len(stages) + SKEW):
            if i < len(stages):
                for f in stages[i][0]:
                    f()
                stages[i][1]()
            if i >= SKEW:
                stages[i - SKEW][2]()

    def proj_phase(s, l):
        A.reset()
        wpa, wpb, wout = pw["wpa"], pw["wpb"], pw["wout"]
        wpa_r, wpb_r, wout_r = pw["wpa_r"], pw["wpb_r"], pw["wout_r"]
        oT = [A.tile(6 * 512, BF16, "oT").rearrange("p (c n) -> p c n", c=6) for _ in range(2)]
        sg = [A.tile(16 * 512, BF16, "sg").rearrange("p (c n) -> p c n", c=16) for _ in range(2)]
        oT_r = [Res("oT0"), Res("oT1")]
        sg_r = [Res("sg0"), Res("sg1")]
        merged = [A.tile(8 * 512, BF16, "merged").rearrange("p (c n) -> p c n", c=8) for _ in range(2)]
        merged_r = [[Res(f"mg{j}_{i}") for i in range(8)] for j in range(2)]
        m1 = [A.tile(512, F32, "m1") for _ in range(2)]
        m2 = [A.tile(512, F32, "m2") for _ in range(2)]
        m1_r = [Res("m10"), Res("m11")]
        m2_r = [Res("m20"), Res("m21")]

        def load_chunk(tc):
            b = tc % 2
            ch = slice(tc * 512, (tc + 1) * 512)
            dma("sp", oT[b][:], o_d[s].rearrange("f p n -> p f n")[:, :, ch], o_res[s], [oT_r[b]])
            dma("sp", sg[b][:, 0:8, :], sg_d[s, 0:8].rearrange("f p n -> p f n")[:, :, ch], sg_res[s][0:8], [sg_r[b]])
            dma("sp", sg[b][:, 8:16, :], sg_d[s, 8:16].rearrange("f p n -> p f n")[:, :, ch], sg_res[s][8:16], [sg_r[b]])
        load_chunk(0)
        k = 0
        pend = []
        for tc in range(4):
            b = tc % 2
            mi = tc % 2
            ch = slice(tc * 512, (tc + 1) * 512)
            for dt in range(8):
                ba = nbank()
                for c in range(4):
                    mm(ps[:, ba, :], wpa[:, c, dt * 128:(dt + 1) * 128], oT[b][:, c, :], c == 0, c == 3,
                       [wpa_r, oT_r[b]], [PB[ba]])
                bb = nbank()
                for c in range(2):
                    mm(ps[:, bb, :], wpb[:, c, dt * 128:(dt + 1) * 128], oT[b][:, 4 + c, :], c == 0, c == 1,
                       [wpb_r, oT_r[b]], [PB[bb]])
                i = k % 2
                k += 1
                tt("dve", m1[i], ps[:, ba, :], sg[b][:, dt, :], ALU.mult, [PB[ba], sg_r[b]], [m1_r[i]])
                tt("dve", m2[i], ps[:, bb, :], sg[b][:, 8 + dt, :], ALU.mult, [PB[bb], sg_r[b]], [m2_r[i]])
                tt("pool", merged[mi][:, dt, :], m1[i], m2[i], ALU.add, [m1_r[i], m2_r[i]], [merged_r[mi][dt]])

            def wout_part(mi=mi, ch=ch, tc=tc):
                for dt in range(8):
                    bo = nbank()
                    for kc in range(8):
                        mm(ps[:, bo, :], wout[:, kc, dt * 128:(dt + 1) * 128], merged[mi][:, kc, :], kc == 0, kc == 7,
                           [wout_r, merged_r[mi][kc]], [PB[bo]])
                    tt("dve", xres[:, dt, ch], ps[:, bo, :], xres[:, dt, ch], ALU.add, [PB[bo], xres_r[dt][tc]],
                       [xres_r[dt][tc]])
            while pend:
                pend.pop(0)()
            if tc + 1 < 4:
                load_chunk(tc + 1)
            pend.append(wout_part)
        while pend:
            pend.pop(0)()

    def ffn(s, l):
        moe = (l == 1)
        gi = 1 if l == 0 else 3
        A.reset()
        if moe:
            cT = A.tile(S, F32, "cT")
            cT_r = [Res(f"cT{i}") for i in range(4)]
        wg = [A.tile(8 * 512, BF16, "wg").rearrange("p (c n) -> p c n", c=8) for _ in range(2)]
        wu = [A.tile(8 * 512, BF16, "wu").rearrange("p (c n) -> p c n", c=8) for _ in range(2)]
        wd = [A.tile(4 * D, BF16, "wd").rearrange("p (c n) -> p c n", c=4) for _ in range(2)]
        wg_r = [Res("wg0"), Res("wg1")]
        wu_r = [Res("wu0"), Res("wu1")]
        wd_r = [Res("wd0"), Res("wd1")]
        jobs = []
        if moe:
            for e_ in range(NEXP):
                for j in range(DFFE // 512):
                    jobs.append((e_, j * 512, 512))
        else:
            for j in range(5):
                jobs.append((None, j * 512, 512))
            jobs.append((None, 2560, 256))

        def load_job(ji):
            e_, f0, w = jobs[ji]
            b = ji % 2
            if moe:
                gsrc = W["w_gate_e_1"][e_, :, f0:f0 + w]
                usrc = W["w_up_e_1"][e_, :, f0:f0 + w]
                dsrc = W["w_down_e_1"][e_, f0:f0 + w, :]
            else:
                gsrc = W["w_gate_0"][:, f0:f0 + w]
                usrc = W["w_up_0"][:, f0:f0 + w]
                dsrc = W["w_down_0"][f0:f0 + w, :]
            dma("pool", wg[b][:, :, 0:w], gsrc.rearrange("(c p) n -> p c n", p=128), [], [wg_r[b]])
            dma("pool", wu[b][:, :, 0:w], usrc.rearrange("(c p) n -> p c n", p=128), [], [wu_r[b]])
            dma("pool", wd[b][:, 0:w // 128, :], dsrc.rearrange("(c p) n -> p c n", p=128), [], [wd_r[b]])
        load_job(0)
        mark = A.off
        sqb = [A.tile(8 * 512, BF16, "sqb").rearrange("p (c n) -> p c n", c=8) for _ in range(2)]
        sqb_r = [Res("sq0"), Res("sq1")]
        rs = [A.tile(512, F32, "rs") for _ in range(2)]
        rs_r = [Res("rs0"), Res("rs1")]
        rstd = [A.tile(512, F32, "rstd") for _ in range(2)]
        rstd_r = [Res("rstd0"), Res("rstd1")]
        if moe:
            h32 = A.tile(8 * 512, F32, "h32").rearrange("p (c n) -> p c n", c=8)
            h32_r = [Res(f"h32_{i}") for i in range(8)]
            wr = A.tile(64, F32, "wr").rearrange("p (c n) -> p c n", c=8)
            wr_r = Res("wr")
            dma("sp", wr[:], W["w_router_1"].rearrange("(c p) n -> p c n", p=128), [], [wr_r])
            lg = A.tile(32, F32, "lg"); lg_r = Res("lg")
            tp = A.tile(32, F32, "tp"); tp_r = Res("tp")
            df = A.tile(4, F32, "df"); df_r = Res("df")
            w2 = A.tile(4, F32, "w2"); w2_r = Res("w2")
            w1 = A.tile(4, F32, "w1"); w1_r = Res("w1")
            c1 = A.tile(8, F32, "c1"); c1_r = Res("c1")
            cpad = [A.tile(128, F32, "cpad") for _ in range(2)]
            cpad_r = [Res("cpad0"), Res("cpad1")]
            for i in range(2):
                P.add("pool", lambda e, i=i: e.memset(cpad[i][:], 0.0), W=[cpad_r[i]])
        kk = 0
        for tc in range(4):
            b = tc % 2
            ch = slice(tc * 512, (tc + 1) * 512)
            act(sqb[b][:], xres[:, :, ch], AF.Square, [xres_r[fc][tc] for fc in range(8)], [sqb_r[b]])
            bk = nbank()
            for fc in range(8):
                mm(ps[:, bk, :], onesb[:], sqb[b][:, fc, :], fc == 0, fc == 7, [onesb_r, sqb_r[b]], [PB[bk]])
            act(rs[b], ps[:, bk, :], AF.Ln, [PB[bk], epsc_r], [rs_r[b]], scale=1.0 / D, bias=epsc[:, 0:1])
            act(rstd[b], rs[b], AF.Exp, [rs_r[b]], [rstd_r[b]], scale=-0.5)
            for fc in range(8):
                if not moe:
                    P.add("dve", lambda e, fc=fc, ch=ch, b=b: e.scalar_tensor_tensor(
                        out=hT[:, fc, ch], in0=xres[:, fc, ch], scalar=gcols[:, gi, fc:fc + 1], in1=rstd[b],
                        op0=ALU.mult, op1=ALU.mult),
                        R=[xres_r[fc][tc], gcols_r, rstd_r[b]], W=[hT_r[fc][tc]])
                else:
                    P.add("dve", lambda e, fc=fc, ch=ch, b=b: e.scalar_tensor_tensor(
                        out=h32[:, fc, :], in0=xres[:, fc, ch], scalar=gcols[:, gi, fc:fc + 1], in1=rstd[b],
                        op0=ALU.mult, op1=ALU.mult),
                        R=[xres_r[fc][tc], gcols_r, rstd_r[b]], W=[h32_r[fc]])
                    cp("pool", hT[:, fc, ch], h32[:, fc, :], [h32_r[fc]], [hT_r[fc][tc]])
            if moe:
                bk = nbank()
                for tl in range(4):
                    for kc in range(8):
                        mm(ps[:, bk, tl * 8:(tl + 1) * 8], h32[:, kc, tl * 128:(tl + 1) * 128], wr[:, kc, :],
                           kc == 0, kc == 7, [h32_r[kc], wr_r], [PB[bk]])
                cp("dve", lg, ps[:, bk, 0:32], [PB[bk]], [lg_r])
                for tl in range(4):
                    P.add("dve", lambda e, tl=tl: e.max(out=tp[:, tl * 8:(tl + 1) * 8], in_=lg[:, tl * 8:(tl + 1) * 8]),
                          R=[lg_r], W=[tp_r])
                tp3 = tp.rearrange("p (t n) -> p t n", t=4)
                tt("dve", df, tp3[:, :, 1], tp3[:, :, 0], ALU.subtract, [tp_r], [df_r])
                act(w2, df, AF.Sigmoid, [df_r], [w2_r])
                P.add("dve", lambda e: e.tensor_scalar(out=w1, in0=w2, scalar1=-1.0, scalar2=1.0,
                                                       op0=ALU.mult, op1=ALU.add), R=[w2_r], W=[w1_r])
                for tl in range(4):
                    t = tc * 4 + tl
                    i = kk % 2
                    kk += 1
                    P.add("dve", lambda e, tl=tl: e.tensor_scalar(
                        out=c1, in0=lg[:, tl * 8:(tl + 1) * 8], scalar1=tp[:, tl * 8:tl * 8 + 1],
                        scalar2=w1[:, tl:tl + 1], op0=ALU.is_equal, op1=ALU.mult),
                        R=[lg_r, tp_r, w1_r], W=[c1_r])
                    P.add("dve", lambda e, tl=tl, i=i: e.tensor_scalar(
                        out=cpad[i][:, 0:8], in0=lg[:, tl * 8:(tl + 1) * 8], scalar1=tp[:, tl * 8 + 1:tl * 8 + 2],
                        scalar2=w2[:, tl:tl + 1], op0=ALU.is_equal, op1=ALU.mult),
                        R=[lg_r, tp_r, w2_r], W=[cpad_r[i]])
                    tt("dve", cpad[i][:, 0:8], cpad[i][:, 0:8], c1, ALU.add, [cpad_r[i], c1_r], [cpad_r[i]])
                    bk2 = nbank()
                    mm(ps[:, bk2, 0:128], cpad[i], ident32, True, True, [cpad_r[i], cf_r], [PB[bk2]])
                    cp("act", cT[:, t * 128:(t + 1) * 128], ps[:, bk2, 0:128], [PB[bk2]], [cT_r[tc]])
        P.barrier()
        A.off = mark
        gT = [A.tile(4 * 512, BF16, "gT").rearrange("p (c n) -> p c n", c=4) for _ in range(2)]
        gT_r = [[Res(f"g{i}_{c}") for c in range(4)] for i in range(2)]
        sa = [A.tile(512, BF16, "sa") for _ in range(2)]
        sa_r = [Res("sa0"), Res("sa1")]
        if moe:
            tmp = [A.tile(512, F32, "tmp") for _ in range(2)]
            tmp_r = [Res("tmp0"), Res("tmp1")]
            cbc = [A.tile(S, F32, "cbc") for _ in range(2)]
            cbc_r = [[Res(f"cbc{i}_{c}") for c in range(4)] for i in range(2)]
        k = 0
        pend = []
        for ji, (e_, f0, w) in enumerate(jobs):
            b = ji % 2
            nf = w // 128
            if moe and f0 == 0:
                cb = e_ % 2
                for tc in range(4):
                    bk = nbank()
                    mm(ps[:, bk, :], sel[:, e_, :], cT[:, tc * 512:(tc + 1) * 512], True, True, [cf_r, cT_r[tc]], [PB[bk]])
                    cp("act", cbc[cb][:, tc * 512:(tc + 1) * 512], ps[:, bk, :], [PB[bk]], [cbc_r[cb][tc]])
            for tc in range(4):
                ch = slice(tc * 512, (tc + 1) * 512)
                gi_ = k % 2
                k += 1
                for fl in range(nf):
                    ba = nbank()
                    for kc in range(8):
                        mm(ps[:, ba, :], wg[b][:, kc, fl * 128:(fl + 1) * 128], hT[:, kc, ch], kc == 0, kc == 7,
                           [wg_r[b], hT_r[kc][tc]], [PB[ba]])
                    bb = nbank()
                    for kc in range(8):
                        mm(ps[:, bb, :], wu[b][:, kc, fl * 128:(fl + 1) * 128], hT[:, kc, ch], kc == 0, kc == 7,
                           [wu_r[b], hT_r[kc][tc]], [PB[bb]])
                    si = fl % 2
                    act(sa[si], ps[:, ba, :], AF.Silu, [PB[ba]], [sa_r[si]])
                    if moe:
                        tt("dve", tmp[si], ps[:, bb, :], cbc[e_ % 2][:, ch], ALU.mult, [PB[bb], cbc_r[e_ % 2][tc]],
                           [tmp_r[si]])
                        tt("dve", gT[gi_][:, fl, :], tmp[si], sa[si], ALU.mult, [tmp_r[si], sa_r[si]],
                           [gT_r[gi_][fl]])
                    else:
                        tt("dve", gT[gi_][:, fl, :], ps[:, bb, :], sa[si], ALU.mult, [PB[bb], sa_r[si]],
                           [gT_r[gi_][fl]])

                def down(b=b, nf=nf, gi_=gi_, ch=ch, tc=tc):
                    for dt in range(8):
                        bo = nbank()
                        for fl in range(nf):
                            mm(ps[:, bo, :], wd[b][:, fl, dt * 128:(dt + 1) * 128], gT[gi_][:, fl, :], fl == 0,
                               fl == nf - 1, [wd_r[b], gT_r[gi_][fl]], [PB[bo]])
                        tt("dve", xres[:, dt, ch], ps[:, bo, :], xres[:, dt, ch], ALU.add, [PB[bo], xres_r[dt][tc]],
                           [xres_r[dt][tc]])
                while pend:
                    pend.pop(0)()
                if tc == 0 and ji + 1 < len(jobs):
                    load_job(ji + 1)
                pend.append(down)
        while pend:
            pend.pop(0)()

    stages = []
    for s in range(nseq):
        stages.append(("load", s, 0))
        for l in range(2):
            stages += [("norm", s, l), ("inproj", s, l), ("attn", s, l), ("proj", s, l), ("ffn", s, l)]
        stages.append(("out", s, 0))
    for (kind, s, l) in stages:
        if kind == "load":
            load_x(s)
        elif kind == "norm":
            pass
        elif kind == "inproj":
            in_proj(s, l)
        elif kind == "attn":
            attention(s, l)
        elif kind == "proj":
            proj_phase(s, l)
        elif kind == "ffn":
            ffn(s, l)
        elif kind == "out":
            final_out(s)
        dump_x(s, f"{kind}{l}")
        if stop_after is not None and (kind, s, l) == tuple(stop_after):
            break
    P.barrier()

    from contextlib import ExitStack
    with ExitStack() as es:
        csem = {e: es.enter_context(nc.semaphore(f"c_{e}")) for e in Prog.ENG}
        dsem = {q: [es.enter_context(nc.semaphore(f"d_{q}{i}")) for i in range(NDSEM)] for q in Prog.DMAQ}
        P.finalize(nc, csem, dsem)
        block = es.enter_context(nc.Block())

        @block.tensor
        def _(e):
            P.emit_engine("pe", e)

        @block.scalar
        def _(e):
            P.emit_engine("act", e)

        @block.vector
        def _(e):
            P.emit_engine("dve", e)

        @block.gpsimd
        def _(e):
            P.emit_engine("pool", e)

        @block.sync
        def _(e):
            P.emit_engine("sp", e)
    return nc


def kernel(**inputs):
    rope, cbf, cf32 = host_consts()
    x = np.ascontiguousarray(inputs["x"], dtype=np.float32)
    nc = build()
    in_maps = []
    for c in range(NCORES):
        m = {}
        for k, v in inputs.items():
            if k == "x":
                m["x"] = x[c * NSEQ:(c + 1) * NSEQ]
            else:
                m[k] = np.ascontiguousarray(v, dtype=np.float32)
        m["c_rope"] = rope
        m["c_bf"] = cbf
        m["c_f32"] = cf32
        in_maps.append(m)
    res = run_bass_kernel_spmd(nc, in_maps, core_ids=list(range(NCORES)))
    return np.concatenate([r["y"] for r in res.results], axis=0).astype(np.float32)
```
